# Optimizing a Trainium2 kernel written in Bass

```python
import jax, jax.numpy as jnp
from jax import lax
import numpy as np

D_MODEL = 1024
BATCH = 32
SEQ = 2048
DEPTH = 4

HG_HEADS = 8
HG_DK = D_MODEL // HG_HEADS
HG_DV = D_MODEL // HG_HEADS
HG_WIDTH = HG_HEADS * HG_DK
HG_CHUNK = 32
S5_WIDTH = D_MODEL
S5_GROUP = 16
S5_GROUPS = S5_WIDTH // S5_GROUP
S5_STATE = 64
S5_DT_MIN = 1e-3
S5_DT_MAX = 1e-1
PEER_HEADS = 8
PEER_NKEYS = 128
PEER_EXPERTS = PEER_NKEYS * PEER_NKEYS
PEER_QDIM = 256
PEER_HALF = PEER_QDIM // 2
PEER_TOPK = 16
PEER_BLOCK = 128
IN_COLS = 4 * HG_WIDTH + S5_WIDTH + 2 * D_MODEL
N_MOD = 6
EPS = 1e-6

kernel_name = "hybrid_hgrn2_s5_peer_adaln"


def rms_norm(x, g):
    x32 = x.astype(jnp.float32)
    y = x32 * lax.rsqrt(jnp.mean(x32 * x32, axis=-1, keepdims=True) + EPS)
    return (y * g.astype(jnp.float32)).astype(x.dtype)


def modulate(h, shift, scale):
    return h * (1.0 + scale[:, None, :]) + shift[:, None, :]


def hgrn2_branch(q_pre, f_pre, i_in, og, lb, norm_g):
    dt = q_pre.dtype
    B_, L = q_pre.shape[0], q_pre.shape[1]
    n_chunks = L // HG_CHUNK
    f32 = f_pre.astype(jnp.float32)
    q = jax.nn.silu(q_pre.astype(jnp.float32))
    log_f = jnp.logaddexp(jnp.log(lb), jnp.log1p(-lb) + jax.nn.log_sigmoid(f32))
    k = (1.0 - lb) * jax.nn.sigmoid(-f32)
    v = i_in.astype(jnp.float32)

    def to_chunks(t, d):
        return t.reshape(B_, n_chunks, HG_CHUNK, HG_HEADS, d).transpose(1, 0, 3, 2, 4)

    qs, ks, gs = to_chunks(q, HG_DK), to_chunks(k, HG_DK), to_chunks(log_f, HG_DK)
    vs = to_chunks(v, HG_DV)
    causal = jnp.tril(jnp.ones((HG_CHUNK, HG_CHUNK), dtype=bool))[:, :, None]

    def step(S, inp):
        qc, kc, vc, gc = inp
        b = jnp.cumsum(gc, axis=-2)
        inter = jnp.einsum('bhck,bhkv->bhcv', qc * jnp.exp(b), S)
        diff = b[:, :, :, None, :] - b[:, :, None, :, :]
        decay = jnp.where(causal, jnp.exp(jnp.where(causal, diff, 0.0)), 0.0)
        att = jnp.einsum('bhtk,bhsk,bhtsk->bhts', qc, kc, decay)
        intra = jnp.einsum('bhts,bhsv->bhtv', att, vc)
        b_last = b[:, :, -1:, :]
        S_new = jnp.exp(b_last[:, :, 0, :])[..., None] * S + jnp.einsum(
            'bhsk,bhsv->bhkv', kc * jnp.exp(b_last - b), vc)
        return S_new, inter + intra

    S0 = jnp.zeros((B_, HG_HEADS, HG_DK, HG_DV), jnp.float32)
    _, o = lax.scan(step, S0, (qs, ks, vs, gs))
    o = o.transpose(1, 0, 3, 2, 4).reshape(B_, L, HG_HEADS, HG_DV)
    o = o * lax.rsqrt(jnp.mean(o * o, axis=-1, keepdims=True) + EPS) * norm_g.astype(jnp.float32)
    o = o.reshape(B_, L, HG_HEADS * HG_DV) * jax.nn.silu(og.astype(jnp.float32))
    return o.astype(dt)


def s5_branch(u, a_re, a_im, log_dt, b_re, b_im, c_re, c_im, d_skip):
    dt_in = u.dtype
    B_, L = u.shape[0], u.shape[1]
    ug = u.astype(jnp.float32).reshape(B_, L, S5_GROUPS, S5_GROUP)
    a_re, a_im = a_re.astype(jnp.float32), a_im.astype(jnp.float32)
    b_re, b_im = b_re.astype(jnp.float32), b_im.astype(jnp.float32)
    c_re, c_im = c_re.astype(jnp.float32), c_im.astype(jnp.float32)
    step = jnp.exp(log_dt.astype(jnp.float32))[:, None]
    mag = jnp.exp(step * a_re)
    ang = step * a_im
    ab_re, ab_im = mag * jnp.cos(ang), mag * jnp.sin(ang)
    den = a_re * a_re + a_im * a_im
    z_re = ((ab_re - 1.0) * a_re + ab_im * a_im) / den
    z_im = (ab_im * a_re - (ab_re - 1.0) * a_im) / den
    bb_re = z_re[..., None] * b_re - z_im[..., None] * b_im
    bb_im = z_re[..., None] * b_im + z_im[..., None] * b_re
    bu_re = jnp.einsum('blgc,gpc->blgp', ug, bb_re)
    bu_im = jnp.einsum('blgc,gpc->blgp', ug, bb_im)
    ar = jnp.broadcast_to(ab_re, bu_re.shape)
    ai = jnp.broadcast_to(ab_im, bu_im.shape)

    def combine(e1, e2):
        a1r, a1i, b1r, b1i = e1
        a2r, a2i, b2r, b2i = e2
        return (a1r * a2r - a1i * a2i,
                a1r * a2i + a1i * a2r,
                a2r * b1r - a2i * b1i + b2r,
                a2r * b1i + a2i * b1r + b2i)

    _, _, xr, xi = lax.associative_scan(combine, (ar, ai, bu_re, bu_im), axis=1)
    y = (jnp.einsum('blgp,gcp->blgc', xr, c_re) - jnp.einsum('blgp,gcp->blgc', xi, c_im)
         + d_skip.astype(jnp.float32).reshape(S5_GROUPS, S5_GROUP) * ug)
    return y.reshape(B_, L, S5_WIDTH).astype(dt_in)


def peer_layer(h, wq, subkeys, u_tab, v_tab):
    B_, L, D = h.shape
    dt = h.dtype
    n_blocks = (B_ * L) // PEER_BLOCK
    blocks = h.reshape(n_blocks, PEER_BLOCK, D)
    keys32 = subkeys.astype(jnp.float32)

    def peer_block(hb):
        q = (hb @ wq).astype(jnp.float32).reshape(PEER_BLOCK, PEER_HEADS, 2, PEER_HALF)
        s = jnp.einsum('thpd,hpnd->thpn', q, keys32)
        s1_v, s1_i = lax.top_k(s[:, :, 0, :], PEER_TOPK)
        s2_v, s2_i = lax.top_k(s[:, :, 1, :], PEER_TOPK)
        cand_v = (s1_v[..., :, None] + s2_v[..., None, :]).reshape(PEER_BLOCK, PEER_HEADS, PEER_TOPK * PEER_TOPK)
        cand_i = (s1_i[..., :, None] * PEER_NKEYS + s2_i[..., None, :]).reshape(PEER_BLOCK, PEER_HEADS, PEER_TOPK * PEER_TOPK)
        top_v, top_j = lax.top_k(cand_v, PEER_TOPK)
        idx = jnp.take_along_axis(cand_i, top_j, axis=-1)
        gate = jax.nn.softmax(top_v, axis=-1)
        u_sel = jnp.take(u_tab, idx, axis=0)
        v_sel = jnp.take(v_tab, idx, axis=0)
        act = jax.nn.gelu(jnp.einsum('thkd,td->thk', u_sel, hb).astype(jnp.float32))
        return jnp.einsum('thk,thkd->td', (gate * act).astype(dt), v_sel)

    out = lax.map(peer_block, blocks)
    return out.reshape(B_, L, D)


def setup_inputs(seed: int = 0) -> dict:
    key = jax.random.key(seed)
    ks = jax.random.split(key, 26)
    f = jnp.float32
    nrm = lambda k, shape, s: jax.random.normal(k, shape, f) * s
    n_idx = jnp.arange(S5_STATE, dtype=f)
    inp = {}
    inp['x'] = nrm(ks[0], (BATCH, SEQ, D_MODEL), 1.0)
    inp['c'] = nrm(ks[1], (BATCH, D_MODEL), 1.0)
    inp['ada_w'] = nrm(ks[2], (DEPTH, D_MODEL, N_MOD * D_MODEL), 0.5 * D_MODEL ** -0.5)
    inp['ada_b'] = nrm(ks[3], (DEPTH, N_MOD * D_MODEL), 0.02)
    inp['norm1_g'] = 1.0 + nrm(ks[4], (DEPTH, D_MODEL), 0.02)
    inp['norm2_g'] = 1.0 + nrm(ks[5], (DEPTH, D_MODEL), 0.02)
    inp['w_in'] = nrm(ks[6], (DEPTH, D_MODEL, IN_COLS), D_MODEL ** -0.5)
    inp['hg_lb_logits'] = nrm(ks[7], (DEPTH, HG_WIDTH), 0.5)
    inp['hg_norm_g'] = 1.0 + nrm(ks[8], (DEPTH, HG_DV), 0.02)
    inp['s5_a_re'] = -0.5 * (1.0 + nrm(ks[9], (DEPTH, S5_GROUPS, S5_STATE), 0.05))
    inp['s5_a_im'] = jnp.pi * n_idx + nrm(ks[10], (DEPTH, S5_GROUPS, S5_STATE), 0.01)
    inp['s5_log_dt'] = jax.random.uniform(ks[11], (DEPTH, S5_GROUPS), f,
                                          np.log(S5_DT_MIN).astype(np.float32), np.log(S5_DT_MAX).astype(np.float32))
    inp['s5_b_re'] = nrm(ks[12], (DEPTH, S5_GROUPS, S5_STATE, S5_GROUP), (2.0 * S5_GROUP) ** -0.5)
    inp['s5_b_im'] = nrm(ks[13], (DEPTH, S5_GROUPS, S5_STATE, S5_GROUP), (2.0 * S5_GROUP) ** -0.5)
    inp['s5_c_re'] = nrm(ks[14], (DEPTH, S5_GROUPS, S5_GROUP, S5_STATE), (2.0 * S5_STATE) ** -0.5)
    inp['s5_c_im'] = nrm(ks[15], (DEPTH, S5_GROUPS, S5_GROUP, S5_STATE), (2.0 * S5_STATE) ** -0.5)
    inp['s5_d'] = nrm(ks[16], (DEPTH, S5_WIDTH), 1.0)
    inp['glu_w'] = nrm(ks[17], (DEPTH, S5_WIDTH, S5_WIDTH), S5_WIDTH ** -0.5)
    inp['glu_b'] = nrm(ks[18], (DEPTH, S5_WIDTH), 0.02)
    inp['w_out'] = nrm(ks[19], (DEPTH, D_MODEL, D_MODEL), D_MODEL ** -0.5)
    inp['peer_wq'] = nrm(ks[20], (DEPTH, D_MODEL, PEER_HEADS * PEER_QDIM), D_MODEL ** -0.5)
    inp['peer_subkeys'] = nrm(ks[21], (DEPTH, PEER_HEADS, 2, PEER_NKEYS, PEER_HALF), PEER_HALF ** -0.5)
    inp['peer_u'] = nrm(ks[22], (DEPTH, PEER_EXPERTS, D_MODEL), D_MODEL ** -0.5)
    inp['peer_v'] = nrm(ks[23], (DEPTH, PEER_EXPERTS, D_MODEL), PEER_HEADS ** -0.5)
    inp['final_g'] = 1.0 + nrm(ks[24], (D_MODEL,), 0.02)
    return inp


def reference(x, c, ada_w, ada_b, norm1_g, norm2_g, w_in, hg_lb_logits, hg_norm_g,
              s5_a_re, s5_a_im, s5_log_dt, s5_b_re, s5_b_im, s5_c_re, s5_c_im, s5_d,
              glu_w, glu_b, w_out, peer_wq, peer_subkeys, peer_u, peer_v, final_g):
    lb_cum = jnp.cumsum(jax.nn.softmax(hg_lb_logits.astype(jnp.float32), axis=0), axis=0)
    lb_all = lb_cum - lb_cum[0:1]
    c_act = jax.nn.silu(c)
    splits = [HG_WIDTH, 2 * HG_WIDTH, 3 * HG_WIDTH, 4 * HG_WIDTH,
              4 * HG_WIDTH + S5_WIDTH, 4 * HG_WIDTH + S5_WIDTH + D_MODEL]
    for l in range(DEPTH):
        mod = c_act @ ada_w[l] + ada_b[l]
        sh1, sc1, g1, sh2, sc2, g2 = jnp.split(mod, N_MOD, axis=-1)
        h = modulate(rms_norm(x, norm1_g[l]), sh1, sc1)
        proj = h @ w_in[l]
        q_pre, f_pre, i_in, og, u_s5, ga, gb = jnp.split(proj, splits, axis=-1)
        y_a = hgrn2_branch(q_pre, f_pre, i_in, og, lb_all[l], hg_norm_g[l])
        y_s = jax.nn.gelu(s5_branch(u_s5, s5_a_re[l], s5_a_im[l], s5_log_dt[l], s5_b_re[l], s5_b_im[l],
                                    s5_c_re[l], s5_c_im[l], s5_d[l]))
        y_b = y_s * jax.nn.sigmoid(y_s @ glu_w[l] + glu_b[l])
        merged = jax.nn.sigmoid(ga) * y_a + jax.nn.sigmoid(gb) * y_b
        x = x + g1[:, None, :] * (merged @ w_out[l])
        h2 = modulate(rms_norm(x, norm2_g[l]), sh2, sc2)
        x = x + g2[:, None, :] * peer_layer(h2, peer_wq[l], peer_subkeys[l], peer_u[l], peer_v[l])
    return rms_norm(x, final_g)
```

```python
import numpy as np
import concourse.bass as bass
import concourse.mybir as mybir

F32 = mybir.dt.float32
BF16 = mybir.dt.bfloat16
U32 = mybir.dt.uint32
I32 = mybir.dt.int32
AF = mybir.ActivationFunctionType
ALU = mybir.AluOpType
AX = mybir.AxisListType

SEM_LIMIT = 30000
N_DMA_SEMS = 12


class Trk:
    __slots__ = ("name", "w", "r", "parent", "kids")

    def __init__(self, name="", parent=None):
        self.name = name
        self.w = None
        self.r = []
        self.parent = parent
        self.kids = {}

    def sub(self, key):
        k = self.kids.get(key)
        if k is None:
            k = Trk(f"{self.name}.{key}", self)
            self.kids[key] = k
        return k

    def related(self):
        out = [self]
        p = self.parent
        while p is not None:
            out.append(p)
            p = p.parent
        stack = list(self.kids.values())
        while stack:
            k = stack.pop()
            out.append(k)
            stack.extend(k.kids.values())
        return out


class Sched:
    ENGS = ("pe", "act", "dve", "pool", "sp")

    def __init__(self, nc, ctx):
        self.nc = nc
        self.ctx = ctx
        self.q = {e: [] for e in self.ENGS}
        self.semidx = 0
        self.cur = {}
        self.cnt = {}
        for e in ("pe", "act", "dve", "pool"):
            self._new_eng_sem(e)
        self.dma_pool = {}
        self.dma_rr = {}
        for e in ("sp", "pool", "act"):
            self.dma_pool[e] = [self._new_dma_sem() for i in range({"sp": N_DMA_SEMS, "pool": 3, "act": 2}[e])]
            self.dma_rr[e] = 0
        self.known = {e: {} for e in self.ENGS}
        self.sems = {}
        self.n_inst = 0

    def _alloc_sem(self, name):
        self.semidx += 1
        h = self.ctx.enter_context(self.nc.semaphore(f"{name}_{self.semidx}"))
        return h

    def _new_eng_sem(self, e):
        h = self._alloc_sem(f"s_{e}")
        key = f"{e}#{self.semidx}"
        self.cur[e] = (h, key)
        self.cnt[e] = 0

    def _new_dma_sem(self):
        h = self._alloc_sem("s_dma")
        return [h, f"dma#{self.semidx}", 0]

    def _deps(self, eng, reads, writes):
        waits = {}

        def need(t, kind):
            if t is None:
                return
            teng, h, key, val = t
            if teng == eng:
                if eng == "pe":
                    return
            if self.known[eng].get(key, 0) >= val:
                return
            cur = waits.get(key)
            if cur is None or cur[1] < val:
                waits[key] = (h, val)

        for trk in reads:
            for t in trk.related():
                need(t.w, "raw")
        for trk in writes:
            for t in trk.related():
                need(t.w, "waw")
                for rt in t.r:
                    need(rt, "war")
        for key, (h, val) in waits.items():
            self.known[eng][key] = val
        return list(waits.values())

    def _post(self, ticket, reads, writes):
        for trk in writes:
            trk.w = ticket
            trk.r = []
            stack = list(trk.kids.values())
            while stack:
                k = stack.pop()
                k.w = None
                k.r = []
                stack.extend(k.kids.values())
        for trk in reads:
            trk.r = [x for x in trk.r if x[2] != ticket[2]] + [ticket]

    def op(self, eng, fn, reads=(), writes=(), drain=False):
        assert eng in ("pe", "act", "dve", "pool")
        waits = self._deps(eng, reads, writes)
        if drain and self.cnt[eng] > 0:
            h0, key0 = self.cur[eng]
            if self.known[eng].get(key0, 0) < self.cnt[eng]:
                waits.append((h0, self.cnt[eng]))
                self.known[eng][key0] = self.cnt[eng]
        if self.cnt[eng] >= SEM_LIMIT:
            self._new_eng_sem(eng)
        self.cnt[eng] += 1
        h, key = self.cur[eng]
        ticket = (eng, h, key, self.cnt[eng])
        self.q[eng].append((waits, fn, h, 1))
        self._post(ticket, reads, writes)
        self.n_inst += 1
        return ticket

    def dma(self, fn, reads=(), writes=(), eng="sp"):
        waits = self._deps(eng, reads, writes)
        pool = self.dma_pool[eng]
        rr = self.dma_rr[eng]
        slot = pool[rr]
        if slot[2] + 16 > SEM_LIMIT:
            slot = self._new_dma_sem()
            pool[rr] = slot
        self.dma_rr[eng] = (rr + 1) % len(pool)
        h, key, val = slot
        if val > 0 and self.known[eng].get(key, 0) < val:
            waits.append((h, val))
            self.known[eng][key] = val
        slot[2] = val + 16
        ticket = ("dma", h, key, val + 16)
        self.q[eng].append((waits, fn, h, 16))
        self._post(ticket, reads, writes)
        self.n_inst += 1
        return ticket

    def wait_all(self, eng, trks):
        waits = self._deps(eng, trks, ())
        self.q[eng].append((waits, None, None, 0))

    def replay(self, block):
        nc = self.nc
        qs = self.q

        def run(engine, lst):
            for waits, fn, h, inc in lst:
                for (sh, val) in waits:
                    engine.wait_ge(sh, val)
                if fn is not None:
                    inst = fn(engine)
                    inst.then_inc(h, inc)

        @block.tensor
        def _(e):
            run(e, qs["pe"])

        @block.scalar
        def _(e):
            run(e, qs["act"])

        @block.vector
        def _(e):
            run(e, qs["dve"])

        @block.gpsimd
        def _(e):
            run(e, qs["pool"])

        @block.sync
        def _(e):
            run(e, qs["sp"])


import numpy as np
from contextlib import ExitStack

P = 128
D = 1024
NT = 1024
SEQ = 2048
NHB = SEQ // NT
DEPTH = 4
EPS = 1e-6
IN_COLS = 7168


class R:
    __slots__ = ("ap", "trk")

    def __init__(self, ap, trk):
        self.ap = ap
        self.trk = trk


class Buf:
    def __init__(self, k, name, shape, dt, psum=False):
        nc = k.nc
        self.shape = list(shape)
        self.dt = dt
        self.t = k.ctx.enter_context((nc.psum_tensor if psum else nc.sbuf_tensor)(name, list(shape), dt))
        self.trk = Trk(name)
        self.row = int(np.prod(shape[1:]))

    def __getitem__(self, idx):
        return R(self.t[idx], self.trk)

    def sub(self, key, idx):
        return R(self.t[idx], self.trk.sub(key))

    def ap(self, p0, npart, off, dims, key=None):
        a = bass.AP(self.t, p0 * self.row + off, [[self.row, npart]] + [list(d) for d in dims])
        return R(a, self.trk if key is None else self.trk.sub(key))


class Dram:
    def __init__(self, k, name, shape, dt, kind=None):
        if kind is None:
            self.t = k.nc.dram_tensor(name, list(shape), dt)
        else:
            self.t = k.nc.dram_tensor(name, list(shape), dt, kind=kind)
        self.a = self.t.ap()
        self.trk = Trk(name)
        self.shape = list(shape)

    def __getitem__(self, idx):
        return R(self.a[idx], self.trk)

    def r(self, ap, key=None):
        return R(ap, self.trk if key is None else self.trk.sub(key))


def _v(x):
    return x.ap if isinstance(x, R) else x


def _t(*xs):
    return [x.trk for x in xs if isinstance(x, R)]


class KB:
    def _pe_sig(self, out, lhsT):
        la, oa = lhsT.ap, out.ap
        sig = (la.offset // la.ap[0][0], la.ap[0][1], oa.offset // oa.ap[0][0])
        prev = getattr(self, "_pe_prev", None)
        self._pe_prev = sig
        if prev is not None and prev != sig:
            self.n_drain = getattr(self, "n_drain", 0) + 1
            return True
        return False

    def mm(self, out, lhsT, rhs, start=True, stop=True, skip=False):
        dr = self._pe_sig(out, lhsT)
        self.S.op("pe", lambda e: e.matmul(out.ap, lhsT=lhsT.ap, rhs=rhs.ap, start=start, stop=stop, skip_group_check=skip),
                  reads=_t(lhsT, rhs), writes=_t(out), drain=dr)

    def tr(self, out, in_, ident):
        dr = self._pe_sig(out, in_)
        self.S.op("pe", lambda e: e.transpose(out.ap, in_.ap, ident.ap), reads=_t(in_, ident), writes=_t(out), drain=dr)

    def act(self, out, in_, func, bias=None, scale=None, eng="act"):
        kw = {}
        if bias is not None:
            kw["bias"] = _v(bias)
        if scale is not None:
            kw["scale"] = _v(scale)
        self.S.op("act", lambda e: e.activation(out=out.ap, in_=in_.ap, func=func, **kw),
                  reads=_t(in_, bias, scale), writes=_t(out))

    def tt(self, out, a, b, op, eng="dve"):
        self.S.op(eng, lambda e: e.tensor_tensor(out=out.ap, in0=a.ap, in1=b.ap, op=op), reads=_t(a, b), writes=_t(out))

    def ts(self, out, a, s1, s2, op0, op1=None, eng="dve"):
        if op1 is None:
            self.S.op(eng, lambda e: e.tensor_scalar(out=out.ap, in0=a.ap, scalar1=_v(s1), scalar2=None, op0=op0),
                      reads=_t(a, s1), writes=_t(out))
        else:
            self.S.op(eng, lambda e: e.tensor_scalar(out=out.ap, in0=a.ap, scalar1=_v(s1), scalar2=_v(s2), op0=op0, op1=op1),
                      reads=_t(a, s1, s2), writes=_t(out))

    def stt(self, out, a, scal, b, op0, op1):
        self.S.op("dve", lambda e: e.scalar_tensor_tensor(out=out.ap, in0=a.ap, scalar=_v(scal), in1=b.ap, op0=op0, op1=op1),
                  reads=_t(a, scal, b), writes=_t(out))

    def copy(self, out, in_, eng="dve"):
        if eng == "act":
            self.S.op("act", lambda e: e.copy(out=out.ap, in_=in_.ap), reads=_t(in_), writes=_t(out))
        else:
            self.S.op(eng, lambda e: e.tensor_copy(out=out.ap, in_=in_.ap), reads=_t(in_), writes=_t(out))

    def scan(self, out, d0, d1, init, op0, op1):
        self.S.op("dve", lambda e: e.tensor_tensor_scan(out=out.ap, data0=d0.ap, data1=d1.ap, initial=_v(init), op0=op0, op1=op1),
                  reads=_t(d0, d1, init), writes=_t(out))

    def memset(self, out, val, eng="dve"):
        self.S.op(eng, lambda e: e.memset(out.ap, val), reads=[], writes=_t(out))

    def reduce(self, out, in_, op, axis=AX.X):
        self.S.op("dve", lambda e: e.tensor_reduce(out=out.ap, in_=in_.ap, axis=axis, op=op), reads=_t(in_), writes=_t(out))

    def recip(self, out, in_):
        self.S.op("dve", lambda e: e.reciprocal(out=out.ap, in_=in_.ap), reads=_t(in_), writes=_t(out))

    def iota(self, out, pattern, base=0, cm=0):
        self.S.op("pool", lambda e: e.iota(out.ap, pattern, base=base, channel_multiplier=cm), reads=[], writes=_t(out))

    def dma(self, out, in_, eng="sp", slow=False):
        if slow:
            self.S.dma(lambda e: e.dma_start(out=out.ap, in_=in_.ap, allow_slow_non_contiguous=True), reads=_t(in_), writes=_t(out), eng=eng)
        else:
            self.S.dma(lambda e: e.dma_start(out=out.ap, in_=in_.ap), reads=_t(in_), writes=_t(out), eng=eng)


class View:
    def __init__(self, ap, trk):
        self.a = ap
        self.trk = trk
        self.row = ap.ap[0][0]

    def __getitem__(self, idx):
        return R(self.a[idx], self.trk)

    def sub(self, key, idx):
        return R(self.a[idx], self.trk.sub(key))

    def ap(self, p0, npart, off, dims, key=None):
        a = bass.AP(self.a.tensor, self.a.offset + p0 * self.row + off, [[self.row, npart]] + [list(d) for d in dims])
        return R(a, self.trk if key is None else self.trk.sub(key))


class Arena:
    SLOT_W = 4096

    def __init__(self, k, nslots):
        self.n = nslots
        self.t = k.ctx.enter_context(k.nc.sbuf_tensor("arena", [P, nslots * self.SLOT_W], F32))
        self.trks = [Trk(f"slot{i}") for i in range(nslots)]

    def view(self, slot, dt, shape, byte_off=0, nslots=1, key=None):
        nel = int(np.prod(shape))
        w0 = slot * self.SLOT_W + byte_off // 4
        if dt == F32 or dt == I32 or dt == U32:
            nw = nel
            base = self.t[:, w0:w0 + nw]
            if dt != F32:
                base = base.bitcast(dt)
        else:
            nw = (nel + 1) // 2
            base = self.t[:, w0:w0 + nw].bitcast(dt)
        assert byte_off + nw * 4 <= nslots * self.SLOT_W * 4, (slot, shape, byte_off)
        if len(shape) > 1:
            names = " ".join(f"d{i}" for i in range(len(shape)))
            kw = {f"d{i}": int(shape[i]) for i in range(len(shape))}
            base = base.rearrange(f"p ({names}) -> p {names}", **kw)
        nbytes = nw * 4
        if nslots > 1:
            trk = _MultiTrk([self.trks[slot + i] for i in range(nslots)])
        elif byte_off == 0 and nbytes == self.SLOT_W * 4:
            trk = self.trks[slot]
        else:
            g0, g1 = byte_off // 1024, (byte_off + nbytes + 1023) // 1024
            trk = _MultiTrk([self.trks[slot].sub(g) for g in range(g0, g1)])
        return View(base, trk)


class _MultiTrk:
    def __init__(self, lst):
        self.lst = lst


_old_t = _t


def _t(*xs):
    out = []
    for x in xs:
        if isinstance(x, R):
            if isinstance(x.trk, _MultiTrk):
                out.extend(x.trk.lst)
            elif isinstance(x.trk, (list, tuple)):
                out.extend(x.trk)
            else:
                out.append(x.trk)
    return out


def _kb_ext():
    def vmax(self, out, in_):
        self.S.op("dve", lambda e: e.max(out=out.ap, in_=in_.ap), reads=_t(in_), writes=_t(out))

    def vmaxidx(self, out, in_max, in_values):
        self.S.op("dve", lambda e: e.max_index(out=out.ap, in_max=in_max.ap, in_values=in_values.ap), reads=_t(in_max, in_values), writes=_t(out))

    def vmatchrep(self, out, in_to_replace, in_values, imm):
        self.S.op("dve", lambda e: e.match_replace(out=out.ap, in_to_replace=in_to_replace.ap, in_values=in_values.ap, imm_value=imm),
                  reads=_t(in_to_replace, in_values), writes=_t(out))
    KB.vmax, KB.vmaxidx, KB.vmatchrep = vmax, vmaxidx, vmatchrep


_kb_ext()


import os
S5STOP = int(os.environ.get('S5STOP', '99'))
PSTOP = float(os.environ.get('PSTOP', '99'))

TWO_PI = float(2 * np.pi)


class MK(KB):
    def __init__(self, nseq, layer_ids, do_peer=True, taps=(), n_hb=NHB, final_norm=True, sel_lb=False):
        self.final_norm = final_norm
        self.sel_lb = sel_lb
        self.nseq = nseq
        self.layer_ids = list(layer_ids)
        self.L = len(layer_ids)
        self.only_final = (self.L == 0)
        if self.only_final:
            self.L = 1
        self.do_peer = do_peer
        self.taps = {}
        self.tap_names = list(taps)
        self.n_hb = n_hb
        self.nc = bass.Bass("TRN2", target_bir_lowering=False)
        self.ctx = ExitStack()

    def declare_io(self):
        L, ns = self.L, self.nseq
        def din(name, shape):
            return Dram(self, name, shape, F32, kind="ExternalInput")
        self.tapd = {}
        if self.only_final:
            self.x = din("x", [ns, SEQ, D])
            self.final_g = din("final_g", [D])
            self.y = Dram(self, "y", [ns, SEQ, D], F32, kind="ExternalOutput")
            return
        if self.sel_lb:
            self.lsel = din("lsel", [DEPTH])
        self.x = din("x", [ns, SEQ, D])
        self.c = din("c", [ns, D])
        self.ada_w = din("ada_w", [L, D, 6 * D])
        self.ada_b = din("ada_b", [L, 6 * D])
        self.norm1_g = din("norm1_g", [L, D])
        self.norm2_g = din("norm2_g", [L, D])
        self.w_in = din("w_in", [L, D, IN_COLS])
        self.hg_lb_logits = din("hg_lb_logits", [DEPTH, D])
        self.hg_norm_g = din("hg_norm_g", [L, 128])
        self.s5_a_re = din("s5_a_re", [L, 64, 64])
        self.s5_a_im = din("s5_a_im", [L, 64, 64])
        self.s5_log_dt = din("s5_log_dt", [L, 64])
        self.s5_b_re = din("s5_b_re", [L, 64, 64, 16])
        self.s5_b_im = din("s5_b_im", [L, 64, 64, 16])
        self.s5_c_re = din("s5_c_re", [L, 64, 16, 64])
        self.s5_c_im = din("s5_c_im", [L, 64, 16, 64])
        self.s5_d = din("s5_d", [L, D])
        self.glu_w = din("glu_w", [L, D, D])
        self.glu_b = din("glu_b", [L, D])
        self.w_out = din("w_out", [L, D, D])
        self.peer_wq = din("peer_wq", [L, D, 2048])
        self.peer_subkeys = din("peer_subkeys", [L, 8, 2, 128, 128])
        if self.do_peer:
            self.peer_u = din("peer_u", [L, 16384, D])
            self.peer_v = din("peer_v", [L, 16384, D])
        self.final_g = din("final_g", [D])
        self.y = Dram(self, "y", [ns, SEQ, D], F32, kind="ExternalOutput")
        self.sc_min = Dram(self, "sc_min", [L, P, 64 * 2 * 64], BF16)
        self.sc_R = Dram(self, "sc_R", [L, P, 32 * 2 * 128], BF16)
        self.sc_mi = Dram(self, "sc_mi", [L, P, 64 * 128], BF16)
        if self.do_peer:
            self.sc_ut = Dram(self, "sc_ut", [L, 128, P, 8 * 128], BF16)
            self.sc_v = Dram(self, "sc_v", [L, 128, P, D], BF16)
        self.tapd = {}
        for (nm, shape, dt) in self.tap_names:
            self.tapd[nm] = Dram(self, "tap_" + nm, shape, dt, kind="ExternalOutput")

    def alloc(self):
        self.S = Sched(self.nc, self.ctx)
        L, ns = self.L, self.nseq
        self.xT = Buf(self, "xT", [P, 8, NT], F32)
        self.S32 = Buf(self, "S32", [P, L, 8, 128], F32)
        self.Sbf = Buf(self, "Sbf", [P, 8, 128], BF16)
        self.s5st = Buf(self, "s5st", [P, L, 64], F32)
        self.s5c = Buf(self, "s5c", [P, L, 4, 32], F32)
        self.identf = Buf(self, "identf", [P, P], F32)
        self.identb = Buf(self, "identb", [P, P], BF16)
        self.onesb = Buf(self, "onesb", [P, P], BF16)
        self.iotaf = Buf(self, "iotaf", [P, P], F32)
        self.pidx = Buf(self, "pidx", [P, 1], F32)
        self.cmask = Buf(self, "cmask", [P, P], F32)
        self.scanm = Buf(self, "scanm", [P, NT], BF16)
        self.modv = Buf(self, "modv", [P, L, 48, ns], F32)
        self.gs = Buf(self, "gs", [P, L, 2, 8, ns], F32)
        self.lb = Buf(self, "lb", [P, DEPTH, 8], F32)
        self.oml = Buf(self, "oml", [P, DEPTH, 8], F32)
        self.ng = Buf(self, "ng", [P, L, 2, 8], F32)
        self.fing = Buf(self, "fing", [P, 8], F32)
        self.hgn = Buf(self, "hgn", [P, L], F32)
        self.glub = Buf(self, "glub", [P, L, 8], F32)
        self.ar = Arena(self, 8)
        self.ps = Buf(self, "ps", [P, 8, 512], F32, psum=True)
        self._rr = 0

    def pbank(self, lo=4, hi=8):
        b = lo + (self._rr % (hi - lo))
        self._rr += 1
        return b

    def psf(self, b, shape=None):
        a = self.ps.t[:, b, :]
        if shape is not None:
            names = " ".join(f"d{i}" for i in range(len(shape)))
            kw = {f"d{i}": int(shape[i]) for i in range(len(shape))}
            a = a.rearrange(f"p ({names}) -> p {names}", **kw)
        return View(a, self.ps.trk.sub(b))

    def psb(self, b, shape=None):
        a = self.ps.t[:, b, :].bitcast(BF16)
        if shape is not None:
            names = " ".join(f"d{i}" for i in range(len(shape)))
            kw = {f"d{i}": int(shape[i]) for i in range(len(shape))}
            a = a.rearrange(f"p ({names}) -> p {names}", **kw)
        return View(a, self.ps.trk.sub(b))

    def consts(self):
        ar = self.ar
        ti = ar.view(0, I32, [P])
        tp = ar.view(0, I32, [1], byte_off=1024)
        self.iota(ti[:, :], [[1, P]], base=0, cm=0)
        self.copy(self.iotaf[:, :], ti[:, :])
        self.iota(tp[:, :], [[0, 1]], base=0, cm=1)
        self.copy(self.pidx[:, :], tp[:, :])
        self.ts(self.identf[:, :], self.iotaf[:, :], self.pidx[:, 0:1], None, ALU.is_equal)
        self.copy(self.identb[:, :], self.identf[:, :])
        self.memset(self.onesb[:, :], 1.0)
        tb = ar.view(0, I32, [P], byte_off=2048)
        self.iota(tb[:, :], [[1, 4], [0, 32]], base=0, cm=0)
        tbf = ar.view(0, F32, [P], byte_off=4096)
        self.copy(tbf[:, :], tb[:, :])
        pb = ar.view(0, I32, [1], byte_off=3072)
        self.S.op("dve", lambda e: e.tensor_scalar(out=pb[:, :].ap, in0=tp[:, :].ap, scalar1=5, scalar2=None,
                                                   op0=ALU.arith_shift_right), reads=_t(tp[:, :]), writes=_t(pb[:, :]))
        pbf = ar.view(0, F32, [1], byte_off=3584)
        self.copy(pbf[:, :], pb[:, :])
        m1 = ar.view(0, F32, [P], byte_off=8192)
        self.ts(m1[:, :], self.iotaf[:, :], self.pidx[:, 0:1], None, ALU.is_ge)
        self.ts(self.cmask[:, :], tbf[:, :], pbf[:, 0:1], None, ALU.is_equal)
        self.tt(self.cmask[:, :], self.cmask[:, :], m1[:, :], ALU.mult)
        self.bm = Buf(self, "bm", [P, 4], F32)
        self.ts(self.bm[:, :], self.iotaf[:, 0:4], pbf[:, 0:1], None, ALU.is_equal)
        sm = ar.view(1, I32, [NT])
        self.iota(sm[:, :], [[0, 32], [1, 32]], base=0, cm=0)
        smf = ar.view(2, F32, [NT])
        self.copy(smf[:, :], sm[:, :])
        self.ts(self.scanm[:, :], smf[:, :], 0.5, None, ALU.is_gt)
        self.imk = Buf(self, "imk", [P, P], F32)
        f16 = ar.view(0, I32, [P], byte_off=10240)
        self.iota(f16[:, :], [[1, 8], [0, 16]], base=0, cm=0)
        f16f = ar.view(0, F32, [P], byte_off=11264)
        self.copy(f16f[:, :], f16[:, :])
        p16 = ar.view(0, I32, [1], byte_off=12288)
        self.S.op("dve", lambda e: e.tensor_scalar(out=p16[:, :].ap, in0=tp[:, :].ap, scalar1=4, scalar2=None,
                                                   op0=ALU.arith_shift_right), reads=_t(tp[:, :]), writes=_t(p16[:, :]))
        p16f = ar.view(0, F32, [1], byte_off=12800)
        self.copy(p16f[:, :], p16[:, :])
        self.ts(self.imk[:, :], f16f[:, :], p16f[:, 0:1], None, ALU.is_ge)

    def prologue_small(self):
        L, ns, ar = self.L, self.nseq, self.ar
        for l in range(L):
            self.dma(self.ng[:, l, 0, :], self.norm1_g.r(self.norm1_g.a[l].rearrange("(c p) -> p c", p=P)), slow=True)
            self.dma(self.ng[:, l, 1, :], self.norm2_g.r(self.norm2_g.a[l].rearrange("(c p) -> p c", p=P)), slow=True)
            self.dma(self.glub[:, l, :], self.glu_b.r(self.glu_b.a[l].rearrange("(c p) -> p c", p=P)), slow=True)
            self.dma(self.hgn[:, l:l + 1], self.hg_norm_g.r(self.hg_norm_g.a[l].rearrange("(p o) -> p o", o=1)), slow=True)
        self.dma(self.fing[:, :], self.final_g.r(self.final_g.a.rearrange("(c p) -> p c", p=P)), slow=True)
        lg = ar.view(0, F32, [DEPTH, 8])
        self.dma(lg[:, :, :], self.hg_lb_logits.r(self.hg_lb_logits.a.rearrange("l (h p) -> p l h", p=P)), slow=True)
        ex = ar.view(0, F32, [DEPTH, 8], byte_off=1024)
        self.act(ex[:, :, :], lg[:, :, :], AF.Exp)
        sm = ar.view(0, F32, [8], byte_off=2048)
        self.tt(sm[:, :], ex[:, 0, :], ex[:, 1, :], ALU.add)
        self.tt(sm[:, :], sm[:, :], ex[:, 2, :], ALU.add)
        self.tt(sm[:, :], sm[:, :], ex[:, 3, :], ALU.add)
        rs = ar.view(0, F32, [8], byte_off=2560)
        self.recip(rs[:, :], sm[:, :])
        self.memset(self.lb[:, 0, :], 0.0)
        for l in range(1, DEPTH):
            t = ar.view(0, F32, [8], byte_off=3072 + 64 * l)
            self.tt(t[:, :], ex[:, l, :], rs[:, :], ALU.mult)
            self.tt(self.lb[:, l, :], self.lb[:, l - 1, :], t[:, :], ALU.add)
        if self.sel_lb:
            selb = ar.view(0, F32, [DEPTH], byte_off=4096)
            self.dma(selb[:, :], self.lsel.r(bass.AP(self.lsel.t, 0, [[0, P], [1, DEPTH]])), slow=True)
            lbs = ar.view(0, F32, [8], byte_off=4608)
            self.ts(lbs[:, :], self.lb[:, 0, :], selb[:, 0:1], None, ALU.mult)
            for l_ in range(1, DEPTH):
                self.stt(lbs[:, :], self.lb[:, l_, :], selb[:, l_:l_ + 1], lbs[:, :], ALU.mult, ALU.add)
            self.copy(self.lb[:, 0, :], lbs[:, :])
        self.ts(self.oml[:, :, :], self.lb[:, :, :], -1.0, 1.0, ALU.mult, ALU.add)
        cT = ar.view(1, F32, [8, ns])
        for bb in range(ns):
            self.dma(cT[:, :, bb], self.c.r(self.c.a[bb].rearrange("(c p) -> p c", p=P)), slow=True)
        cact = ar.view(1, F32, [8, ns], byte_off=2048)
        self.act(cact[:, :, :], cT[:, :, :], AF.Silu)
        abT = ar.view(1, F32, [L, 48], byte_off=4096)
        self.dma(abT[:, :, :], self.ada_b.r(self.ada_b.a.rearrange("l (m p) -> p l m", p=P)), slow=True)
        CB = 512
        nblk = 6 * D // CB
        for l in range(L):
            for blk in range(nblk):
                slot = 2 + (blk % 2)
                wblk = ar.view(slot, F32, [8, CB], key=("adaw", blk % 2))
                src = self.ada_w.a[l, :, blk * CB:(blk + 1) * CB].rearrange("(c p) n -> p c n", p=P)
                self.dma(wblk[:, :, :], self.ada_w.r(src), eng="sp")
                b = self.pbank()
                pv = self.psf(b, [CB // P, 128])
                for ml in range(CB // P):
                    for dc in range(8):
                        self.mm(pv[:, ml, 0:ns], wblk[:, dc, ml * P:(ml + 1) * P], cact[:, dc, :], start=(dc == 0), stop=(dc == 7))
                m0 = blk * (CB // P)
                self.tt(self.modv[:, l, m0:m0 + CB // P, :], pv[:, :, 0:ns],
                        abT.ap(0, P, l * 48 + m0, [[1, CB // P], [0, ns]]), ALU.add)
        for l in range(L):
            for w in range(2):
                msc = 8 * (1 + 3 * w)
                t = ar.view(4, F32, [8, ns])
                self.ts(t[:, :, :], self.modv[:, l, msc:msc + 8, :], 1.0, None, ALU.add)
                self.tt(self.gs[:, l, w, :, :], t[:, :, :], self.ng.ap(0, P, (l * 2 + w) * 8, [[1, 8], [0, ns]]), ALU.mult)

    def s5_prep(self, l):
        ar = self.ar
        V = lambda off, shape=(32,): ar.view(0, F32, list(shape), byte_off=off)
        are, aim, ldt = V(0), V(128), V(256)
        self.dma(are[:, :], self.s5_a_re.r(self.s5_a_re.a[l].rearrange("(q e) p -> (e p) q", e=2)), slow=True)
        self.dma(aim[:, :], self.s5_a_im.r(self.s5_a_im.a[l].rearrange("(q e) p -> (e p) q", e=2)), slow=True)
        for e in range(2):
            src = bass.AP(self.s5_log_dt.t, l * 64 + e, [[0, 64], [2, 32]])
            self.dma(ar.view(0, F32, [32], byte_off=256).ap(64 * e, 64, 0, [[1, 32]]), self.s5_log_dt.r(src), slow=True)
        dt, mag, ang = V(384), V(512), V(640)
        self.act(dt[:, :], ldt[:, :], AF.Exp)
        t0 = V(768)
        self.tt(t0[:, :], dt[:, :], are[:, :], ALU.mult)
        self.act(mag[:, :], t0[:, :], AF.Exp)
        self.tt(ang[:, :], dt[:, :], aim[:, :], ALU.mult)
        ki = ar.view(0, I32, [32], byte_off=896)
        kf = V(1024)
        self.ts(t0[:, :], ang[:, :], 1.0 / TWO_PI, None, ALU.mult)
        self.copy(ki[:, :], t0[:, :])
        self.copy(kf[:, :], ki[:, :])
        r1 = V(1152)
        self.stt(r1[:, :], kf[:, :], -TWO_PI, ang[:, :], ALU.mult, ALU.add)
        sy, hh, cy, sn, cs = V(1280), V(1408), V(1536), V(1664), V(1792)
        self.act(sy[:, :], r1[:, :], AF.Sin, scale=0.5)
        self.act(hh[:, :], r1[:, :], AF.Sin, scale=0.25)
        self.tt(cy[:, :], hh[:, :], hh[:, :], ALU.mult)
        self.ts(cy[:, :], cy[:, :], -2.0, 1.0, ALU.mult, ALU.add)
        self.tt(sn[:, :], sy[:, :], cy[:, :], ALU.mult)
        self.ts(sn[:, :], sn[:, :], 2.0, None, ALU.mult)
        self.tt(cs[:, :], sy[:, :], sy[:, :], ALU.mult)
        self.ts(cs[:, :], cs[:, :], -2.0, 1.0, ALU.mult, ALU.add)
        if S5STOP <= 1:
            return
        pwr = ar.view(0, F32, [17, 32], byte_off=4096)
        pwi = ar.view(0, F32, [17, 32], byte_off=4096 + 17 * 128)
        self.memset(pwr[:, 8, :], 1.0)
        self.memset(pwi[:, 8, :], 0.0)
        self.tt(pwr[:, 9, :], mag[:, :], cs[:, :], ALU.mult)
        self.tt(pwi[:, 9, :], mag[:, :], sn[:, :], ALU.mult)
        im2 = V(1920)
        self.tt(im2[:, :], mag[:, :], mag[:, :], ALU.mult)
        self.recip(im2[:, :], im2[:, :])
        self.tt(pwr[:, 7, :], pwr[:, 9, :], im2[:, :], ALU.mult)
        self.tt(pwi[:, 7, :], pwi[:, 9, :], im2[:, :], ALU.mult)
        self.ts(pwi[:, 7, :], pwi[:, 7, :], -1.0, None, ALU.mult)
        ta, tb = V(2048), V(2176)

        def cmul(or_, oi_, ar_, ai_, br_, bi_):
            self.tt(ta[:, :], ar_, br_, ALU.mult)
            self.tt(tb[:, :], ai_, bi_, ALU.mult)
            self.tt(or_, ta[:, :], tb[:, :], ALU.subtract)
            self.tt(ta[:, :], ar_, bi_, ALU.mult)
            self.tt(tb[:, :], ai_, br_, ALU.mult)
            self.tt(oi_, ta[:, :], tb[:, :], ALU.add)
        for k in range(2, 9):
            cmul(pwr[:, 8 + k, :], pwi[:, 8 + k, :], pwr[:, 7 + k, :], pwi[:, 7 + k, :], pwr[:, 9, :], pwi[:, 9, :])
        for k in range(2, 8):
            cmul(pwr[:, 8 - k, :], pwi[:, 8 - k, :], pwr[:, 9 - k, :], pwi[:, 9 - k, :], pwr[:, 7, :], pwi[:, 7, :])
        self.copy(self.s5c[:, l, 0, :], pwr[:, 16, :])
        self.copy(self.s5c[:, l, 1, :], pwr[:, 16, :])
        self.ts(self.s5c[:, l, 2, :], pwi[:, 16, :], -1.0, None, ALU.mult)
        self.copy(self.s5c[:, l, 3, :], pwi[:, 16, :])
        if S5STOP <= 2:
            return
        den, zr, zi, abm1 = V(2304), V(2432), V(2560), V(2688)
        self.tt(ta[:, :], are[:, :], are[:, :], ALU.mult)
        self.tt(tb[:, :], aim[:, :], aim[:, :], ALU.mult)
        self.tt(den[:, :], ta[:, :], tb[:, :], ALU.add)
        self.recip(den[:, :], den[:, :])
        self.ts(abm1[:, :], pwr[:, 9, :], -1.0, None, ALU.add)
        self.tt(ta[:, :], abm1[:, :], are[:, :], ALU.mult)
        self.tt(tb[:, :], pwi[:, 9, :], aim[:, :], ALU.mult)
        self.tt(zr[:, :], ta[:, :], tb[:, :], ALU.add)
        self.tt(zr[:, :], zr[:, :], den[:, :], ALU.mult)
        self.tt(ta[:, :], pwi[:, 9, :], are[:, :], ALU.mult)
        self.tt(tb[:, :], abm1[:, :], aim[:, :], ALU.mult)
        self.tt(zi[:, :], ta[:, :], tb[:, :], ALU.subtract)
        self.tt(zi[:, :], zi[:, :], den[:, :], ALU.mult)
        W = 32 * 16
        bre = ar.view(1, F32, [32, 16], byte_off=0)
        bim = ar.view(1, F32, [32, 16], byte_off=2048)
        bbr = ar.view(1, F32, [32, 16], byte_off=4096)
        bbi = ar.view(1, F32, [32, 16], byte_off=6144)
        t1 = ar.view(1, F32, [32, 16], byte_off=8192)
        t2 = ar.view(1, F32, [32, 16], byte_off=10240)
        self.dma(bre[:, :, :], self.s5_b_re.r(self.s5_b_re.a[l].rearrange("(q e) p c -> (e p) q c", e=2)), slow=True)
        self.dma(bim[:, :, :], self.s5_b_im.r(self.s5_b_im.a[l].rearrange("(q e) p c -> (e p) q c", e=2)), slow=True)
        zrb = zr.ap(0, P, 0, [[1, 32], [0, 16]])
        zib = zi.ap(0, P, 0, [[1, 32], [0, 16]])
        self.tt(t1[:, :, :], bre[:, :, :], zrb, ALU.mult)
        self.tt(t2[:, :, :], bim[:, :, :], zib, ALU.mult)
        self.tt(bbr[:, :, :], t1[:, :, :], t2[:, :, :], ALU.subtract)
        self.tt(t1[:, :, :], bim[:, :, :], zrb, ALU.mult)
        self.tt(t2[:, :, :], bre[:, :, :], zib, ALU.mult)
        self.tt(bbi[:, :, :], t1[:, :, :], t2[:, :, :], ALU.add)
        Pr = ar.view(2, F32, [32, 8, 16])
        Pi = ar.view(3, F32, [32, 8, 16])
        for j in range(8):
            k = 8 + 7 - j
            pr_b = pwr.ap(0, P, k * 32, [[1, 32], [0, 16]])
            pi_b = pwi.ap(0, P, k * 32, [[1, 32], [0, 16]])
            self.tt(t1[:, :, :], bbr[:, :, :], pr_b, ALU.mult)
            self.tt(t2[:, :, :], bbi[:, :, :], pi_b, ALU.mult)
            self.tt(Pr[:, :, j, :], t1[:, :, :], t2[:, :, :], ALU.subtract)
            self.tt(t1[:, :, :], bbi[:, :, :], pr_b, ALU.mult)
            self.tt(t2[:, :, :], bbr[:, :, :], pi_b, ALU.mult)
            self.tt(Pi[:, :, j, :], t1[:, :, :], t2[:, :, :], ALU.add)
        if S5STOP <= 3:
            return
        cre = ar.view(1, F32, [32, 16], byte_off=0)
        cim = ar.view(1, F32, [32, 16], byte_off=2048)
        for (dst, srcT, slot) in ((cre, self.s5_c_re, 4), (cim, self.s5_c_im, 5)):
            cnat = ar.view(slot, F32, [64, 64])
            src = bass.AP(srcT.t, l * 65536, [[64, 16], [1024, 64], [1, 64]])
            self.dma(cnat.ap(0, 16, 0, [[64, 64], [1, 64]]), srcT.r(src))
            b = self.pbank()
            pv = self.psf(b, [32, 16])
            for q in range(32):
                self.tr(pv[:, q, :], cnat.ap(0, 16, 2 * q * 64, [[1, 128]]), self.identf.ap(0, 16, 0, [[1, 16]]))
            self.copy(dst[:, :, :], pv[:, :, :], eng="act")
        if S5STOP <= 4:
            return
        Rb = ar.view(6, BF16, [32, 2, 8, 16])
        Qr = ar.view(4, F32, [32, 8, 16])
        Qi = ar.view(5, F32, [32, 8, 16])
        for i in range(8):
            for (k, o_r, o_i) in ((8 + i + 1, Rb[:, :, 0, i, :], Rb[:, :, 1, i, :]), (8 + i - 7, Qr[:, :, i, :], Qi[:, :, i, :])):
                pr_b = pwr.ap(0, P, k * 32, [[1, 32], [0, 16]])
                pi_b = pwi.ap(0, P, k * 32, [[1, 32], [0, 16]])
                self.tt(t1[:, :, :], cre[:, :, :], pr_b, ALU.mult)
                self.tt(t2[:, :, :], cim[:, :, :], pi_b, ALU.mult)
                self.tt(o_r, t1[:, :, :], t2[:, :, :], ALU.subtract)
                self.tt(t1[:, :, :], cre[:, :, :], pi_b, ALU.mult)
                self.tt(t2[:, :, :], cim[:, :, :], pr_b, ALU.mult)
                self.tt(t1[:, :, :], t1[:, :, :], t2[:, :, :], ALU.add)
                self.ts(o_i, t1[:, :, :], -1.0, None, ALU.mult)
        self.dma(self.sc_R[l], ar.view(6, BF16, [32 * 2 * 128])[:, :])
        if S5STOP <= 5:
            return
        dd = ar.view(0, F32, [64], byte_off=8192)
        for j in range(8):
            src = bass.AP(self.s5_d.t, l * D, [[1, 16], [16, 64]])
            self.dma(dd.ap(16 * j, 16, 0, [[1, 64]]), self.s5_d.r(src), slow=True)
        Mi = ar.view(7, BF16, [64, 128])
        tmpm = ar.view(0, F32, [128], byte_off=9216)
        Prf = ar.view(2, F32, [32, 128])
        Pif = ar.view(3, F32, [32, 128])
        Qrf = ar.view(4, F32, [32, 128])
        Qif = ar.view(5, F32, [32, 128])
        for g in [2 * q_ + e_ for e_ in range(2) for q_ in range(32)]:
            q, e = g // 2, g % 2
            b = self.pbank()
            pv = self.psf(b)
            self.mm(pv[:, 0:128], Prf.ap(64 * e, 64, q * 128, [[1, 128]]), Qrf.ap(64 * e, 64, q * 128, [[1, 128]]), start=True, stop=False)
            self.mm(pv[:, 0:128], Pif.ap(64 * e, 64, q * 128, [[1, 128]]), Qif.ap(64 * e, 64, q * 128, [[1, 128]]), start=False, stop=True)
            self.tt(tmpm[:, :], pv[:, 0:128], self.imk[:, :], ALU.mult)
            self.stt(Mi[:, g, :], self.identf[:, :], dd[:, g:g + 1], tmpm[:, :], ALU.mult, ALU.add)
        self.dma(self.sc_mi[l], ar.view(7, BF16, [64 * 128])[:, :])
        if S5STOP <= 6:
            return
        Mn = ar.view(6, BF16, [64, 2, 64])
        for e in range(2):
            for q0 in range(0, 32, 4):
                b = self.pbank()
                pv = self.psf(b, [4, 2, 64])
                for qi in range(4):
                    q = q0 + qi
                    for ri, Pf in enumerate((Prf, Pif)):
                        self.mm(pv[:, qi, ri, :], Pf.ap(64 * e, 64, q * 128, [[1, 128]]),
                                self.identf.ap(64 * e, 64, 64 * e, [[1, 64]]))
                self.copy(Mn.ap(0, P, (2 * q0 + e) * 128, [[256, 4], [1, 128]]), pv.ap(0, P, 0, [[128, 4], [1, 128]]), eng="act")
        if os.environ.get("MINMODE", "") != "1":
            self.dma(self.sc_min[l], ar.view(6, BF16, [64 * 2 * 64])[:, :])

    def dbg(self, name, src, shape, dt):
        if os.environ.get("TAPS", "") != "1":
            return
        if name in self.tapd:
            return
        d = Dram(self, "tap_" + name, shape, dt, kind="ExternalOutput")
        self.tapd[name] = d
        self.dma(d[tuple(slice(None) for _ in shape)], src)

    def tap(self, name, src):
        d = self.tapd[name]
        self.dma(d[tuple(slice(None) for _ in d.shape)], src)

    def finish(self):
        outs = [self.y] + list(self.tapd.values())
        self.S.wait_all("sp", [o.trk for o in outs])
        with self.nc.Block() as block:
            self.S.replay(block)
        return self.nc

    def load_x(self, b, hf):
        ar = self.ar
        for tt in range(8):
            xs = ar.view(tt % 2, F32, [D], key=("xs", tt % 2))
            r0 = hf * NT + tt * P
            self.dma(xs[:, :], self.x.r(self.x.a[b, r0:r0 + P, :]))
            for half in range(2):
                bk = self.pbank()
                pv = self.psf(bk, [4, P])
                for j in range(4):
                    dc = half * 4 + j
                    self.tr(pv[:, j, :], xs[:, dc * P:(dc + 1) * P], self.identf[:, :])
                self.copy(self.xT[:, half * 4:half * 4 + 4, tt * P:(tt + 1) * P], pv[:, :, :], eng=("act" if half else "dve"))

    def rms_rstd(self, src, ntok, sq, rstd, nfeat_chunks=8, bank0=0):
        nb = (ntok + 511) // 512
        for dc in range(nfeat_chunks):
            self.act(sq[:, dc, 0:ntok], src[:, dc, 0:ntok], AF.Square)
        for n in range(nb):
            w = min(512, ntok - n * 512)
            pv = self.psf(bank0 + n)
            for dc in range(nfeat_chunks):
                self.mm(pv[:, 0:w], self.onesb[:, :], sq[:, dc, n * 512:n * 512 + w], start=(dc == 0), stop=(dc == nfeat_chunks - 1))
            self.act(rstd[:, n * 512:n * 512 + w], pv[:, 0:w], AF.Ln, scale=1.0 / (nfeat_chunks * P), bias=EPS)
        self.act(rstd[:, 0:ntok], rstd[:, 0:ntok], AF.Exp, scale=-0.5)

    def norm_mod(self, l, which, b, hT, t0, ntok):
        ar = self.ar
        sq = ar.view(5, BF16, [8, NT])
        rstd = ar.view(4, F32, [NT], byte_off=0, key="rstd")
        tmp = [ar.view(4, F32, [NT], byte_off=4096 * (1 + i), key=("ntmp", i)) for i in range(2)]
        xv = View(self.xT.t[:, :, t0:t0 + ntok], self.xT.trk)
        self.rms_rstd(xv, ntok, sq, rstd)
        msh = 24 * which
        for dc in range(8):
            t = tmp[dc % 2]
            self.stt(t[:, 0:ntok], self.xT[:, dc, t0:t0 + ntok], self.gs[:, l, which, dc, b:b + 1], rstd[:, 0:ntok], ALU.mult, ALU.mult)
            self.act(hT[:, dc, 0:ntok], t[:, 0:ntok], AF.Identity, bias=self.modv[:, l, msh + dc, b:b + 1])

    def load_w(self, dst, src_ap, dram):
        self.dma(dst, dram.r(src_ap.rearrange("(c p) n -> p c n", p=P)), eng="pool")

    def mixer(self, l, b):
        ar = self.ar
        li = self.layer_ids[l]
        hT = ar.view(6, BF16, [8, NT])
        mb = ar.view(7, BF16, [8, NT])
        self.norm_mod(l, 0, b, hT, 0, NT)
        self.dbg("xT", self.xT[:, :, :], [P, 8, NT], F32)
        self.dbg("hT", hT[:, :, :], [P, 8, NT], BF16)
        wA = ar.view(0, BF16, [8, D])
        self.load_w(wA[:, :, :], self.w_in.a[l, :, 4096:5120], self.w_in)
        Ublk = ar.view(1, BF16, [64, 8, 16])
        for j in range(8):
            for nb in range(2):
                bk = self.pbank()
                pv = self.psf(bk, [32, 16])
                for dc in range(8):
                    self.mm(pv[:, :, :], hT.ap(0, P, dc * NT + j, [[8, 128]]), wA[:, dc, nb * 512:(nb + 1) * 512], start=(dc == 0), stop=(dc == 7))
                self.copy(Ublk[:, nb * 32:(nb + 1) * 32, j, :], pv[:, :, :], eng=("act" if (j + nb) % 2 else "dve"))
        self.dbg("Ublk", Ublk[:, :, :, :], [P, 64, 8, 16], BF16)
        UT = ar.view(2, BF16, [64, 128])
        for g0 in range(0, 64, 8):
            bk = self.pbank()
            pv = self.psb(bk, [8, 128])
            for gi in range(8):
                self.tr(pv[:, gi, :], Ublk.ap(0, P, (g0 + gi) * 128, [[1, 128]]), self.identb[:, :])
            self.copy(UT[:, g0:g0 + 8, :], pv[:, :, :], eng=("act" if (g0 // 8) % 2 else "dve"))
        Mn = ar.view(3, BF16, [64, 2, 64])
        self.dbg("UT", UT[:, :, :], [P, 64, 128], BF16)
        self.dma(ar.view(3, BF16, [64 * 2 * 64])[:, :], self.sc_min[l])
        Xs = ar.view(1, BF16, [128, 64])
        for q0 in range(0, 32, 4):
            bkr, bki = self.pbank(), self.pbank()
            pr, pi = self.psf(bkr, [4, 128]), self.psf(bki, [4, 128])
            for e in range(2):
                for ql in range(4):
                    g = 2 * (q0 + ql) + e
                    self.mm(pr.ap(64 * e, 64, ql * 128, [[1, 128]]), Mn[:, g, 0, :], UT[:, g, :])
                    self.mm(pi.ap(64 * e, 64, ql * 128, [[1, 128]]), Mn[:, g, 1, :], UT[:, g, :])
            self.copy(Xs.ap(0, P, q0, [[1, 4], [64, 128]]), pr[:, :, :], eng="act")
            self.copy(Xs.ap(0, P, 32 + q0, [[1, 4], [64, 128]]), pi[:, :, :], eng="dve")
        self.dbg("Xs", Xs[:, :, :], [P, 128, 64], BF16)
        hist = ar.view(4, BF16, [128, 64])
        st = View(self.s5st.t[:, l, :], self.s5st.trk)
        sc_t = ar.view(5, F32, [2, 64], key="scan_t")
        AA = View(self.s5c.t[:, l, 0:2, :].rearrange("p a b -> p (a b)"), self.s5c.trk)
        for n in range(128):
            self.copy(hist[:, n, :], st[:, :], eng="pool")
            self.tt(sc_t[:, 0, :], st[:, :], AA[:, :], ALU.mult, eng="pool")
            self.tt(sc_t[:, 1, 0:32], st[:, 32:64], self.s5c[:, l, 2, :], ALU.mult, eng="pool")
            self.tt(sc_t[:, 1, 32:64], st[:, 0:32], self.s5c[:, l, 3, :], ALU.mult, eng="pool")
            self.tt(sc_t[:, 0, :], sc_t[:, 0, :], sc_t[:, 1, :], ALU.add, eng="pool")
            self.tt(st[:, :], sc_t[:, 0, :], Xs[:, n, :], ALU.add, eng="pool")
        Rb = ar.view(5, BF16, [32, 2, 128])
        self.dbg("hist", hist[:, :, :], [P, 128, 64], BF16)
        self.dma(ar.view(5, BF16, [32 * 2 * 128])[:, :], self.sc_R[l])
        Mi = ar.view(3, BF16, [64, 128])
        self.dma(ar.view(3, BF16, [64 * 128])[:, :], self.sc_mi[l])
        Ys = ar.view(1, BF16, [8, D])
        for g0 in range(0, 64, 4):
            bks = [((g0 // 4) % 2) * 4 + gi for gi in range(4)]
            pvs = [self.psf(bk) for bk in bks]
            for gi in range(4):
                g = g0 + gi
                self.mm(pvs[gi][:, 0:128], UT[:, g, :], Mi[:, g, :], start=True, stop=False)
            for e in range(2):
                for gi in range(e, 4, 2):
                    g = g0 + gi
                    q = g // 2
                    self.mm(pvs[gi][:, 0:128], hist.ap(64 * e, 64, q, [[64, 128]]), Rb.ap(64 * e, 64, (q * 2 + 0) * 128, [[1, 128]]), start=False, stop=False)
                    self.mm(pvs[gi][:, 0:128], hist.ap(64 * e, 64, 32 + q, [[64, 128]]), Rb.ap(64 * e, 64, (q * 2 + 1) * 128, [[1, 128]]), start=False, stop=True)
            for gi in range(4):
                self.act(Ys.ap(0, P, (g0 + gi) * 16, [[D, 8], [1, 16]]),
                         self.psf(bks[gi], [4, 8, 16])[:, 0, :, :], AF.Gelu_apprx_tanh)
        self.dbg("Ys", Ys[:, :, :], [P, 8, D], BF16)
        ysT = ar.view(2, BF16, [8, NT])
        for chc in range(8):
            bk = self.pbank()
            pv = self.psb(bk, [8, 128])
            for i in range(8):
                self.tr(pv[:, i, :], Ys[:, i, chc * P:(chc + 1) * P], self.identb[:, :])
            self.copy(ysT.ap(0, P, chc * NT, [[1, 8], [8, 128]]), pv[:, :, :], eng=("act" if chc % 2 else "dve"))
        self.dbg("ysT", ysT[:, :, :], [P, 8, NT], BF16)
        wB = ar.view(0, BF16, [8, D])
        self.load_w(wB[:, :, :], self.w_in.a[l, :, 6144:7168], self.w_in)
        for cho in range(8):
            for nb in range(2):
                bk = self.pbank()
                pv = self.psf(bk)
                for dc in range(8):
                    self.mm(pv[:, :], wB[:, dc, cho * P:(cho + 1) * P], hT[:, dc, nb * 512:(nb + 1) * 512], start=(dc == 0), stop=(dc == 7))
                self.act(mb[:, cho, nb * 512:(nb + 1) * 512], pv[:, :], AF.Sigmoid)
        self.dbg("mb_gb", mb[:, :, :], [P, 8, NT], BF16)
        wG = ar.view(3, BF16, [8, D])
        self.load_w(wG[:, :, :], self.glu_w.a[l], self.glu_w)
        sg = [ar.view(4, BF16, [512], byte_off=1024 * i, key=("sg", i)) for i in range(2)]
        k = 0
        for cho in range(8):
            for nb in range(2):
                bk = self.pbank()
                pv = self.psf(bk)
                for chc in range(8):
                    self.mm(pv[:, :], wG[:, chc, cho * P:(cho + 1) * P], ysT[:, chc, nb * 512:(nb + 1) * 512], start=(chc == 0), stop=(chc == 7))
                s = sg[k % 2]
                k += 1
                self.act(s[:, :], pv[:, :], AF.Sigmoid, bias=self.glub[:, l, cho:cho + 1])
                self.tt(s[:, :], s[:, :], ysT[:, cho, nb * 512:(nb + 1) * 512], ALU.mult)
                self.tt(mb[:, cho, nb * 512:(nb + 1) * 512], mb[:, cho, nb * 512:(nb + 1) * 512], s[:, :], ALU.mult)
        self.dbg("mb_glu", mb[:, :, :], [P, 8, NT], BF16)
        wV = ar.view(0, BF16, [8, D])
        self.load_w(wV[:, :, :], self.w_in.a[l, :, 2048:3072], self.w_in)
        vall = ar.view(1, BF16, [8, D])
        for tt in range(8):
            for nb in range(2):
                bk = self.pbank()
                pv = self.psf(bk)
                for dc in range(8):
                    self.mm(pv[:, :], hT[:, dc, tt * P:(tt + 1) * P], wV[:, dc, nb * 512:(nb + 1) * 512], start=(dc == 0), stop=(dc == 7))
                self.copy(vall[:, tt, nb * 512:(nb + 1) * 512], pv[:, :], eng=("act" if (tt + nb) % 2 else "dve"))
        self.dbg("vall", vall[:, :, :], [P, 8, D], BF16)
        for h in range(8):
            self.hgrn2_head(l, li, b, h, hT, mb, vall)
        self.dbg("mb_fin", mb[:, :, :], [P, 8, NT], BF16)
        wO = ar.view(0, BF16, [8, D])
        self.load_w(wO[:, :, :], self.w_out.a[l], self.w_out)
        for dco in range(8):
            for nb in range(2):
                bk = self.pbank()
                pv = self.psf(bk)
                for chc in range(8):
                    self.mm(pv[:, :], wO[:, chc, dco * P:(dco + 1) * P], mb[:, chc, nb * 512:(nb + 1) * 512], start=(chc == 0), stop=(chc == 7))
                self.stt(self.xT[:, dco, nb * 512:(nb + 1) * 512], pv[:, :], self.modv[:, l, 16 + dco, b:b + 1],
                         self.xT[:, dco, nb * 512:(nb + 1) * 512], ALU.mult, ALU.add)

    def hgrn2_head(self, l, li, b, h, hT, mb, vall):
        ar = self.ar
        par = h % 2
        wH = ar.view(3, BF16, [8, 4, 128], byte_off=8192 * par, key=("wH", par))
        cols = [h * 128, 1024 + h * 128, 3072 + h * 128, 5120 + h * 128]
        for w, c0 in enumerate(cols):
            self.load_w(wH[:, :, w, :], self.w_in.a[l, :, c0:c0 + 128], self.w_in)
        B2 = lambda i, key: ar.view(2, BF16, [NT], byte_off=2048 * i, key=key)
        F4 = lambda i, key: ar.view(4, F32, [NT], byte_off=4096 * i, key=key)
        F5 = lambda i, key: ar.view(5, F32, [NT], byte_off=4096 * i, key=key)
        qeT, keT, kdT = B2(0, "qeT"), B2(1, "keT"), B2(2, "kdT")
        sqo = kdT
        gate = ar.view(5, BF16, [NT], byte_off=12288 + 256, key="gate")
        kdz = ar.view(2, BF16, [8, 4, 128], byte_off=8192, key="kdz")
        attm = [ar.view(2, BF16, [128], byte_off=2048 * 3 + 256 * i, key=("attm", i)) for i in range(2)]
        fT, bT, kT, t1 = F4(0, "fT"), F4(1, "bT"), F4(2, "kT"), F4(3, "t1")
        t2, t3, t4 = F5(0, "t2"), F5(1, "t3"), F5(2, "t4")
        ebl = ar.view(5, F32, [32], byte_off=4096 * 3, key="ebl")

        def proj(w, nb):
            bk = self.pbank()
            pv = self.psf(bk)
            for dc in range(8):
                self.mm(pv[:, :], wH[:, dc, w, :], hT[:, dc, nb * 512:(nb + 1) * 512], start=(dc == 0), stop=(dc == 7))
            return pv
        for nb in range(2):
            sl = slice(nb * 512, (nb + 1) * 512)
            pv = proj(1, nb)
            self.act(t1[:, sl], pv[:, :], AF.Sigmoid)
            pv = proj(0, nb)
            self.act(t2[:, sl], pv[:, :], AF.Silu)
            pv = proj(2, nb)
            self.act(t3[:, sl], pv[:, :], AF.Silu)
            pv = proj(3, nb)
            self.act(t4[:, sl], pv[:, :], AF.Sigmoid)
        self.ts(fT[:, :], t1[:, :], self.oml[:, li, h:h + 1], self.lb[:, li, h:h + 1], ALU.mult, ALU.add)
        self.tt(gate[:, :], t3[:, :], t4[:, :], ALU.mult)
        self.act(t1[:, :], fT[:, :], AF.Ln)
        self.scan(bT[:, :], self.scanm[:, :], t1[:, :], 0.0, ALU.mult, ALU.add)
        self.ts(kT[:, :], fT[:, :], -1.0, 1.0, ALU.mult, ALU.add)
        self.act(t3[:, :], bT[:, :], AF.Exp)
        self.tt(qeT[:, :], t2[:, :], t3[:, :], ALU.mult)
        self.act(t4[:, :], bT[:, :], AF.Exp, scale=-1.0)
        self.tt(t1[:, :], kT[:, :], t4[:, :], ALU.mult)
        self.copy(keT[:, :], t1[:, :], eng="act")
        self.act(ebl[:, :], bT.ap(0, P, 31, [[32, 32]]), AF.Exp)
        self.tt(kdT.ap(0, P, 0, [[32, 32], [1, 32]]), t1.ap(0, P, 0, [[32, 32], [1, 32]]), ebl.ap(0, P, 0, [[1, 32], [0, 32]]), ALU.mult)
        bk = self.pbank()
        pvb = self.psb(bk, [8, 128])
        for tt in range(8):
            self.tr(pvb[:, tt, :], kdT[:, tt * P:(tt + 1) * P], self.identb[:, :])
        self.dbg("fT", fT[:, :], [P, NT], F32)
        self.dbg("bT", bT[:, :], [P, NT], F32)
        self.dbg("qeT", qeT[:, :], [P, NT], BF16)
        self.dbg("keT", keT[:, :], [P, NT], BF16)
        self.dbg("kdT", kdT[:, :], [P, NT], BF16)
        self.dbg("gate", gate[:, :], [P, NT], BF16)
        self.tt(kdz[:, :, :, :], pvb.ap(0, P, 0, [[128, 8], [0, 4], [1, 128]]),
                View(self.bm.t[:, :], self.bm.trk).ap(0, P, 0, [[0, 8], [1, 4], [0, 128]]), ALU.mult)
        S32 = View(self.S32.t[:, l, h, :], self.S32.trk.sub(h))
        Sb = View(self.Sbf.t[:, h, :], self.Sbf.trk.sub(h))
        self.copy(Sb[:, :], S32[:, :], eng="act")
        ops = [self.psf(0), self.psf(1)]
        for tt in range(8):
            ov = ops[tt // 4]
            oc = (tt % 4) * P
            bk = self.pbank()
            av = self.psf(bk)
            self.mm(av[:, 0:128], keT[:, tt * P:(tt + 1) * P], qeT[:, tt * P:(tt + 1) * P])
            am = attm[tt % 2]
            self.tt(am[:, :], av[:, 0:128], self.cmask[:, :], ALU.mult)
            bku = self.pbank()
            uv = self.psf(bku, [4, 128])
            for cc in range(4):
                self.mm(uv[:, cc, :], kdz[:, tt, cc, :], vall[:, tt, h * 128:(h + 1) * 128])
            self.mm(ov[:, oc:oc + P], vall[:, tt, h * 128:(h + 1) * 128], am[:, :], start=True, stop=False)
            for cc in range(4):
                c = tt * 4 + cc
                self.mm(ov[:, oc + 32 * cc:oc + 32 * cc + 32], Sb[:, :], qeT[:, c * 32:(c + 1) * 32], start=False, stop=(cc == 3))
                self.stt(Sb[:, :], S32[:, :], ebl[:, c:c + 1], uv[:, cc, :], ALU.mult, ALU.add)
                self.stt(S32[:, :], S32[:, :], ebl[:, c:c + 1], uv[:, cc, :], ALU.mult, ALU.add)
        rst = t2
        self.copy(t3[:, 0:512], ops[0][:, :], eng="act") if os.environ.get("TAPS", "") == "1" and h == 0 else None
        self.dbg("o0", t3[:, 0:512], [P, 512], F32)
        self.dbg("S32", self.S32[:, l, 0, :], [P, 128], F32)
        for nb in range(2):
            self.act(sqo[:, nb * 512:(nb + 1) * 512], ops[nb][:, :], AF.Square)
        for nb in range(2):
            pv = self.psf(2 + nb)
            self.mm(pv[:, :], self.onesb[:, :], sqo[:, nb * 512:(nb + 1) * 512])
            self.act(rst[:, nb * 512:(nb + 1) * 512], pv[:, :], AF.Ln, scale=1.0 / 128, bias=EPS)
        self.act(rst[:, :], rst[:, :], AF.Exp, scale=-0.5)
        for nb in range(2):
            sl = slice(nb * 512, (nb + 1) * 512)
            self.tt(t3[:, sl], ops[nb][:, :], rst[:, sl], ALU.mult)
        self.stt(t4[:, :], t3[:, :], self.hgn[:, l:l + 1], gate[:, :], ALU.mult, ALU.mult)
        self.tt(mb[:, h, :], mb[:, h, :], t4[:, :], ALU.add)

    def final_store(self, b, hf):
        ar = self.ar
        sq = ar.view(5, BF16, [8, NT])
        rstd = ar.view(4, F32, [NT], byte_off=0, key="rstd")
        if self.final_norm:
            self.rms_rstd(self.xT, NT, sq, rstd)
        for half in range(2):
            yT = ar.view(6 + half, F32, [4, NT])
            for j in range(4):
                dc = half * 4 + j
                if self.final_norm:
                    self.stt(yT[:, j, :], self.xT[:, dc, :], self.fing[:, dc:dc + 1], rstd[:, :], ALU.mult, ALU.mult)
                else:
                    self.copy(yT[:, j, :], self.xT[:, dc, :], eng=("act" if j % 2 else "dve"))
        for tt in range(8):
            ys = ar.view(tt % 2, F32, [D], key=("ys", tt % 2))
            for half in range(2):
                yT = ar.view(6 + half, F32, [4, NT])
                bk = self.pbank()
                pv = self.psf(bk, [4, P])
                for j in range(4):
                    self.tr(pv[:, j, :], yT[:, j, tt * P:(tt + 1) * P], self.identf[:, :])
                self.copy(ys[:, half * 512:(half + 1) * 512], self.psf(bk)[:, :], eng=("act" if half else "dve"))
            r0 = hf * NT + tt * P
            self.dma(self.y.r(self.y.a[b, r0:r0 + P, :]), ys[:, :])

    def build(self):
        self.declare_io()
        self.alloc()
        self.consts()
        if self.only_final:
            self.dma(self.fing[:, :], self.final_g.r(self.final_g.a.rearrange("(c p) -> p c", p=P)), slow=True)
            for b in range(self.nseq):
                for hf in range(self.n_hb):
                    self.load_x(b, hf)
                    self.final_store(b, hf)
            return self.finish()
        self.prologue_small()
        for l in range(self.L):
            self.s5_prep(l)
        if self.do_peer:
            for l in range(self.L):
                self.peer_prep(l)
        for b in range(self.nseq):
            self.memset(self.S32[:, :, :, :], 0.0)
            self.memset(self.s5st[:, :, :], 0.0)
            for hf in range(self.n_hb):
                self.load_x(b, hf)
                for l in range(self.L):
                    self.mixer(l, b)
                    if self.do_peer:
                        self.peer(l, b)
                self.final_store(b, hf)
        return self.finish()


def _peer_ext():
    NEG = -1.0e30
    TB = 256

    def peer_prep(self, l):
        ar = self.ar
        for j in range(128):
            par = j % 2
            ub = ar.view(0 + par, BF16, [D], byte_off=0)
            vb = ar.view(0 + par, BF16, [D], byte_off=2048)
            ut = ar.view(0 + par, BF16, [8, 128], byte_off=4096)
            usrc = bass.AP(self.peer_u.t, (l * 16384 + j) * D, [[128 * D, 128], [1, D]])
            vsrc = bass.AP(self.peer_v.t, (l * 16384 + j) * D, [[128 * D, 128], [1, D]])
            self.dma(ub[:, :], self.peer_u.r(usrc), eng="pool")
            self.dma(vb[:, :], self.peer_v.r(vsrc), eng="pool")
            bk = self.pbank()
            pv = self.psb(bk, [8, 128])
            for dc in range(8):
                self.tr(pv[:, dc, :], ub[:, dc * P:(dc + 1) * P], self.identb[:, :])
            self.copy(ut[:, :, :], pv[:, :, :], eng=("act" if par else "dve"))
            self.dma(self.sc_ut.r(self.sc_ut.a[l, j], key=j), ar.view(0 + par, BF16, [8 * 128], byte_off=4096)[:, :])
            self.dma(self.sc_v.r(self.sc_v.a[l, j], key=j), vb[:, :])

    def peer(self, l, b):
        ar = self.ar
        if PSTOP <= 1:
            return
        kn = ar.view(7, BF16, [16, 128])
        ksrc = self.peer_subkeys.a[l].rearrange("h t n d -> n (h t) d")
        self.dma(kn[:, :, :], self.peer_subkeys.r(ksrc), eng="pool")
        keysT = ar.view(6, BF16, [16, 128], byte_off=12288)
        for h0 in range(0, 16, 8):
            bk = self.pbank()
            pv = self.psb(bk, [8, 128])
            for hi in range(8):
                self.tr(pv[:, hi, :], kn[:, h0 + hi, :], self.identb[:, :])
            self.copy(keysT[:, h0:h0 + 8, :], pv[:, :, :], eng="act")
        for tb in range(NT // TB):
            self.peer_block(l, b, tb * TB, keysT)

    def peer_block(self, l, b, t0, keysT):
        ar = self.ar
        h2T = ar.view(6, BF16, [8, TB], byte_off=0)
        qT = ar.view(6, BF16, [16, TB], byte_off=4096)
        self.norm_mod(l, 1, b, h2T, t0, TB)
        for blk in range(4):
            wq = ar.view(7, BF16, [8, 512], byte_off=8192 * (blk % 2))
            self.load_w(wq[:, :, :], self.peer_wq.a[l, :, blk * 512:(blk + 1) * 512], self.peer_wq)
            for cbl in range(4):
                cb = blk * 4 + cbl
                bk = self.pbank()
                pv = self.psf(bk)
                for dc in range(8):
                    self.mm(pv[:, 0:TB], wq[:, dc, cbl * P:(cbl + 1) * P], h2T[:, dc, :], start=(dc == 0), stop=(dc == 7))
                self.copy(qT[:, cb, :], pv[:, 0:TB], eng=("act" if cb % 2 else "dve"))
        iT = ar.view(4, BF16, [TB], byte_off=8192)
        jT = ar.view(4, BF16, [TB], byte_off=9216)
        wT = ar.view(4, BF16, [TB], byte_off=10240)
        for tt2 in range(TB // P):
            self.peer_topk(tt2, qT, keysT, iT, jT, wT)
        if PSTOP <= 2.8:
            return
        Gbuf = ar.view(0, BF16, [128, TB], nslots=4)
        iotab = ar.view(4, BF16, [128], byte_off=11264)
        self.copy(iotab[:, :], self.iotaf[:, :])
        SUB = 32
        for s0 in range(0, TB, SUB):
            A = ar.view(5, BF16, [SUB, 128], byte_off=0)
            B = ar.view(5, BF16, [SUB, 128], byte_off=8192)
            io_b = iotab.ap(0, P, 0, [[0, SUB], [1, 128]])
            self.tt(B[:, :, :], io_b, jT.ap(0, P, s0, [[1, SUB], [0, 128]]), ALU.is_equal)
            self.tt(A[:, :, :], io_b, iT.ap(0, P, s0, [[1, SUB], [0, 128]]), ALU.is_equal)
            self.tt(A[:, :, :], A[:, :, :], wT.ap(0, P, s0, [[1, SUB], [0, 128]]), ALU.mult)
            for t4 in range(0, SUB, 4):
                bk = self.pbank()
                pv = self.psf(bk, [4, 128])
                for ti in range(4):
                    self.mm(pv[:, ti, :], A[:, t4 + ti, :], B[:, t4 + ti, :])
                self.copy(Gbuf.ap(0, P, s0 + t4, [[1, 4], [TB, 128]]), pv[:, :, :], eng="act")
        if PSTOP <= 3:
            return
        accs = [self.psf(k) for k in range(4)]
        for j in range(128):
            par = j % 2
            UTc = ar.view(7, BF16, [8, 128], byte_off=4096 * par)
            Vc = ar.view(7, BF16, [D], byte_off=4096 * par + 2048)
            ga = ar.view(7, BF16, [TB], byte_off=8192 + 1024 * par)
            Wc = ar.view(7, BF16, [TB], byte_off=8192 + 1024 * par + 512)
            self.dma(ar.view(7, BF16, [8 * 128], byte_off=4096 * par)[:, :], self.sc_ut.r(self.sc_ut.a[l, j], key=j))
            self.dma(Vc[:, :], self.sc_v.r(self.sc_v.a[l, j], key=j))
            bk = self.pbank()
            pv = self.psf(bk)
            for dc in range(8):
                self.mm(pv[:, 0:TB], UTc[:, dc, :], h2T[:, dc, :], start=(dc == 0), stop=(dc == 7))
            self.act(ga[:, :], pv[:, 0:TB], AF.Gelu_apprx_tanh)
            self.tt(Wc[:, :], ga[:, :], Gbuf[:, j, :], ALU.mult)
            for dco in range(8):
                half = dco % 2
                self.mm(accs[dco // 2][:, half * TB:(half + 1) * TB], Vc[:, dco * P:(dco + 1) * P], Wc[:, :],
                        start=(j == 0 and half == 0), stop=(j == 127), skip=True)
        for dco in range(8):
            half = dco % 2
            self.stt(self.xT[:, dco, t0:t0 + TB], accs[dco // 2][:, half * TB:(half + 1) * TB], self.modv[:, l, 40 + dco, b:b + 1],
                     self.xT[:, dco, t0:t0 + TB], ALU.mult, ALU.add)

    def peer_topk(self, tt2, qT, keysT, iT, jT, wT):
        ar = self.ar
        ssb = ar.view(4, F32, [16, 128], byte_off=0)
        pvs = [self.psf(4 + k, [4, 128]) for k in range(4)]
        for hh in range(16):
            self.mm(pvs[hh // 4][:, hh % 4, :], qT[:, hh, tt2 * P:(tt2 + 1) * P], keysT[:, hh, :])
        for k in range(4):
            self.copy(ssb[:, 4 * k:4 * k + 4, :], pvs[k][:, :, :], eng=("act" if k % 2 else "dve"))
        if PSTOP <= 2.1:
            return
        topv = ar.view(4, F32, [16, 16], byte_off=12288)
        topi = ar.view(4, U32, [16, 16], byte_off=13312)
        scr = ar.view(4, F32, [128], byte_off=14336)
        for hh in range(16):
            self.vmax(topv[:, hh, 0:8], ssb[:, hh, :])
            self.vmaxidx(topi[:, hh, 0:8], topv[:, hh, 0:8], ssb[:, hh, :])
            self.vmatchrep(scr[:, :], topv[:, hh, 0:8], ssb[:, hh, :], NEG)
            self.vmax(topv[:, hh, 8:16], scr[:, :])
            self.vmaxidx(topi[:, hh, 8:16], topv[:, hh, 8:16], scr[:, :])
        if PSTOP <= 2.2:
            return
        topif = ar.view(4, F32, [16, 16], byte_off=14848)
        self.copy(topif[:, :, :], topi[:, :, :])
        cand = ar.view(4, F32, [8, 16, 16], byte_off=0)
        tv = View(topv.a.rearrange("p (h t) r -> p h t r", t=2), topv.trk)
        self.tt(cand[:, :, :, :], tv.ap(0, P, 0, [[32, 8], [1, 16], [0, 16]]), tv.ap(0, P, 16, [[32, 8], [0, 16], [1, 16]]), ALU.add)
        cflat = ar.view(4, F32, [8, 256], byte_off=0)
        cv = ar.view(5, F32, [8, 16], byte_off=0)
        ci = ar.view(5, U32, [8, 16], byte_off=512)
        cscr = ar.view(5, F32, [256], byte_off=1024)
        for h in range(8):
            self.vmax(cv[:, h, 0:8], cflat[:, h, :])
            self.vmaxidx(ci[:, h, 0:8], cv[:, h, 0:8], cflat[:, h, :])
            self.vmatchrep(cscr[:, :], cv[:, h, 0:8], cflat[:, h, :], NEG)
            self.vmax(cv[:, h, 8:16], cscr[:, :])
            self.vmaxidx(ci[:, h, 8:16], cv[:, h, 8:16], cscr[:, :])
        if PSTOP <= 2.3:
            return
        r1i = ar.view(5, U32, [8, 16], byte_off=2048)
        r2i = ar.view(5, U32, [8, 16], byte_off=2560)
        self.S.op("dve", lambda e: e.tensor_scalar(out=r1i[:, :, :].ap, in0=ci[:, :, :].ap, scalar1=4, scalar2=None, op0=ALU.logical_shift_right),
                  reads=_t(ci[:, :, :]), writes=_t(r1i[:, :, :]))
        self.S.op("dve", lambda e: e.tensor_scalar(out=r2i[:, :, :].ap, in0=ci[:, :, :].ap, scalar1=15, scalar2=None, op0=ALU.bitwise_and),
                  reads=_t(ci[:, :, :]), writes=_t(r2i[:, :, :]))
        r1f = ar.view(5, F32, [8, 16], byte_off=3072)
        r2f = ar.view(5, F32, [8, 16], byte_off=3584)
        self.copy(r1f[:, :, :], r1i[:, :, :])
        self.copy(r2f[:, :, :], r2i[:, :, :])
        if PSTOP <= 2.4:
            return
        oh = ar.view(5, F32, [8, 16, 16], byte_off=4096)
        tif = View(topif.a.rearrange("p (h t) r -> p h t r", t=2), topif.trk)
        io16 = View(self.iotaf.t[:, 0:16], self.iotaf.trk)
        res = ar.view(5, F32, [3, 8, 16], byte_off=12288)
        for which, rf in ((0, r1f), (1, r2f)):
            self.tt(oh[:, :, :, :], rf.ap(0, P, 0, [[16, 8], [1, 16], [0, 16]]), io16.ap(0, P, 0, [[0, 8], [0, 16], [1, 16]]), ALU.is_equal)
            self.tt(oh[:, :, :, :], oh[:, :, :, :], tif.ap(0, P, which * 16, [[32, 8], [0, 16], [1, 16]]), ALU.mult)
            self.reduce(res[:, which, :, :], oh[:, :, :, :], ALU.add)
        ex = ar.view(5, F32, [8, 16], byte_off=14336)
        self.tt(ex[:, :, :], cv[:, :, :], cv.ap(0, P, 0, [[16, 8], [0, 16]]), ALU.subtract)
        self.act(ex[:, :, :], ex[:, :, :], AF.Exp)
        zs = ar.view(5, F32, [8], byte_off=14848)
        self.reduce(zs[:, :], ex[:, :, :], ALU.add)
        self.recip(zs[:, :], zs[:, :])
        self.tt(res[:, 2, :, :], ex[:, :, :], zs.ap(0, P, 0, [[1, 8], [0, 16]]), ALU.mult)
        if PSTOP <= 2.5:
            return
        resb = ar.view(5, BF16, [3, 128], byte_off=15360)
        self.copy(resb[:, :, :], res.ap(0, P, 0, [[128, 3], [1, 128]]))
        bk = self.pbank()
        pv = self.psb(bk, [8, 128])
        if PSTOP <= 2.6:
            return
        for w in range(3):
            self.tr(pv[:, w, :], resb[:, w, :], self.identb[:, :])
        if PSTOP <= 2.7:
            return
        ijw = ar.view(4, BF16, [3, 512], byte_off=8192)
        self.copy(ijw[:, :, tt2 * P:(tt2 + 1) * P], pv[:, 0:3, :], eng="dve")

    MK.peer_prep, MK.peer, MK.peer_block, MK.peer_topk = peer_prep, peer, peer_block, peer_topk


_peer_ext()


_LAYER_NAMES = ["ada_w", "ada_b", "norm1_g", "norm2_g", "w_in", "hg_norm_g", "s5_a_re", "s5_a_im", "s5_log_dt", "s5_b_re",
                "s5_b_im", "s5_c_re", "s5_c_im", "s5_d", "glu_w", "glu_b", "w_out", "peer_wq", "peer_subkeys", "peer_u", "peer_v"]
_NC_CACHE = {}


def kernel(**inputs):
    from concourse.bass_utils import run_bass_kernel_spmd
    n_cores = 8
    x = np.ascontiguousarray(np.asarray(inputs["x"], dtype=np.float32))
    c = np.asarray(inputs["c"], dtype=np.float32)
    B = x.shape[0]
    nseq = B // n_cores
    depth = np.asarray(inputs["w_in"]).shape[0]
    if ("layer", nseq) not in _NC_CACHE:
        _NC_CACHE[("layer", nseq)] = MK(nseq, [0], do_peer=True, final_norm=False, sel_lb=True).build()
        _NC_CACHE[("final", nseq)] = MK(nseq, [], do_peer=False).build()
    nc_layer = _NC_CACHE[("layer", nseq)]
    nc_final = _NC_CACHE[("final", nseq)]
    lbl = np.ascontiguousarray(np.asarray(inputs["hg_lb_logits"], dtype=np.float32))
    fg = np.ascontiguousarray(np.asarray(inputs["final_g"], dtype=np.float32))
    cs = [np.ascontiguousarray(c[i * nseq:(i + 1) * nseq]) for i in range(n_cores)]
    xs = [np.ascontiguousarray(x[i * nseq:(i + 1) * nseq]) for i in range(n_cores)]
    for l in range(depth):
        shared = {n: np.ascontiguousarray(np.asarray(inputs[n], dtype=np.float32)[l:l + 1]) for n in _LAYER_NAMES}
        sel = np.zeros(depth, np.float32)
        sel[l] = 1.0
        shared["lsel"] = sel
        shared["hg_lb_logits"] = lbl
        shared["final_g"] = fg
        in_maps = []
        for i in range(n_cores):
            m = dict(shared)
            m["x"] = xs[i]
            m["c"] = cs[i]
            in_maps.append(m)
        res = run_bass_kernel_spmd(nc_layer, in_maps, core_ids=list(range(n_cores)))
        xs = [np.ascontiguousarray(np.asarray(r["y"])) for r in res.results]
        del shared, in_maps, res
    res = run_bass_kernel_spmd(nc_final, [{"x": xs[i], "final_g": fg} for i in range(n_cores)], core_ids=list(range(n_cores)))
    return np.concatenate([np.asarray(r["y"]) for r in res.results], axis=0).astype(np.float32)
```
